# Optimizing a Trainium2 kernel written in Bass

```python
import jax, jax.numpy as jnp
from jax import lax
import numpy as np

D_MODEL = 1024
BATCH = 32
SEQ = 2048
DEPTH = 2

RET_HEADS = 4
RET_DK = 128
RET_DV = 256
RET_CHUNK = 128
ROPE_BASE = 10000.0
HGRN_HEADS = 4
HGRN_DK = 128
HGRN_DV = 128
HGRN_CHUNK = 32
D_FF = 4 * D_MODEL
EPS = 1e-6
MIN_F = 1e-30

RET_QK = RET_HEADS * RET_DK
RET_V = RET_HEADS * RET_DV
HGRN_K = HGRN_HEADS * HGRN_DK
HGRN_V = HGRN_HEADS * HGRN_DV
IN_WIDTHS = (RET_QK, RET_QK, RET_V, RET_V, HGRN_K, HGRN_K, HGRN_V, HGRN_V, D_MODEL, D_MODEL)
D_IN = sum(IN_WIDTHS)
IN_SPLITS = tuple(int(s) for s in np.cumsum(IN_WIDTHS)[:-1])

kernel_name = "retnet_hgrn2_gated_hybrid"


def rmsnorm(x, g):
    xf = x.astype(jnp.float32)
    y = xf * lax.rsqrt(jnp.mean(xf * xf, axis=-1, keepdims=True) + EPS)
    return (y * g.astype(jnp.float32)).astype(x.dtype)


def group_rmsnorm(x, g, n_heads):
    B, S, W = x.shape
    xf = x.astype(jnp.float32).reshape(B, S, n_heads, W // n_heads)
    y = xf * lax.rsqrt(jnp.mean(xf * xf, axis=-1, keepdims=True) + EPS)
    return (y.reshape(B, S, W) * g.astype(jnp.float32)).astype(x.dtype)


def to_chunks(t, chunk):
    B, S, H, d = t.shape
    return t.reshape(B, S // chunk, chunk, H, d).transpose(1, 0, 3, 2, 4)


def from_chunks(t):
    N, B, H, C, d = t.shape
    return t.transpose(1, 0, 3, 2, 4).reshape(B, N * C, H * d)


def rotate_every_two(t):
    t1 = t[..., 0::2]
    t2 = t[..., 1::2]
    return jnp.stack((-t2, t1), axis=-1).reshape(t.shape)


def retnet_rotation(t):
    S = t.shape[1]
    angle = 1.0 / (ROPE_BASE ** jnp.linspace(0.0, 1.0, RET_DK // 2, dtype=jnp.float32))
    angle = jnp.repeat(angle, 2)
    phase = jnp.arange(S, dtype=jnp.float32)[:, None] * angle[None, :]
    cos = jnp.cos(phase)[None, :, None, :]
    sin = jnp.sin(phase)[None, :, None, :]
    return t * cos + rotate_every_two(t) * sin


def chunkwise_retention(q, k, v):
    B = q.shape[0]
    C = RET_CHUNK
    log_gamma = jnp.log1p(-jnp.exp2(-5.0 - jnp.arange(RET_HEADS, dtype=jnp.float32)))
    idx = jnp.arange(C, dtype=jnp.float32)
    dist = idx[:, None] - idx[None, :]
    causal = dist >= 0
    intra_decay = jnp.where(causal[None], jnp.exp(log_gamma[:, None, None] * jnp.where(causal, dist, 0.0)[None]), 0.0)
    q_decay = jnp.exp(log_gamma[:, None] * (idx + 1.0)[None])[..., None]
    k_decay = jnp.exp(log_gamma[:, None] * (C - 1.0 - idx)[None])[..., None]
    chunk_decay = jnp.exp(log_gamma * C)[:, None, None]

    def step(state, xs):
        q_c, k_c, v_c = xs
        scores = jnp.einsum('bhtd,bhsd->bhts', q_c, k_c) * intra_decay
        o = jnp.einsum('bhts,bhsv->bhtv', scores, v_c) + jnp.einsum('bhtd,bhdv->bhtv', q_c * q_decay, state)
        state = chunk_decay * state + jnp.einsum('bhsd,bhsv->bhdv', k_c * k_decay, v_c)
        return state, o

    init = jnp.zeros((B, RET_HEADS, RET_DK, RET_DV), jnp.float32)
    _, o = lax.scan(step, init, (to_chunks(q, C), to_chunks(k, C), to_chunks(v, C)))
    return from_chunks(o)


def chunkwise_hgrn2(q, k, i, log_f):
    B = q.shape[0]
    C = HGRN_CHUNK
    causal = jnp.tril(jnp.ones((C, C), dtype=bool))[:, :, None]

    def step(state, xs):
        q_c, k_c, i_c, lf_c = xs
        b = jnp.cumsum(lf_c, axis=2)
        o_inter = jnp.einsum('bhtd,bhdv->bhtv', q_c * jnp.exp(b), state)
        pair = b[:, :, :, None, :] - b[:, :, None, :, :]
        pair = jnp.where(causal, jnp.exp(jnp.where(causal, pair, 0.0)), 0.0)
        attn = jnp.einsum('bhtd,bhsd,bhtsd->bhts', q_c, k_c, pair)
        o = o_inter + jnp.einsum('bhts,bhsv->bhtv', attn, i_c)
        b_last = b[:, :, -1:, :]
        state = jnp.exp(b_last[:, :, 0, :])[..., None] * state + jnp.einsum('bhsd,bhsv->bhdv', k_c * jnp.exp(b_last - b), i_c)
        return state, o

    init = jnp.zeros((B, HGRN_HEADS, HGRN_DK, HGRN_DV), jnp.float32)
    _, o = lax.scan(step, init, (to_chunks(q, C), to_chunks(k, C), to_chunks(i, C), to_chunks(log_f, C)))
    return from_chunks(o)


def hybrid_mixer(h, w_in, ret_gn_g, hgrn_norm_g, lb, w_br_ret, w_br_hgrn, b_merge, w_out):
    B, S, _ = h.shape
    dt = h.dtype
    proj = h @ w_in
    (r_q, r_k, r_v, r_g, g_q, g_f, g_i, g_g, m_ret, m_hg) = jnp.split(proj, IN_SPLITS, axis=-1)

    rq = retnet_rotation(r_q.astype(jnp.float32).reshape(B, S, RET_HEADS, RET_DK))
    rk = retnet_rotation(r_k.astype(jnp.float32).reshape(B, S, RET_HEADS, RET_DK)) * (RET_DK ** -0.5)
    rv = r_v.astype(jnp.float32).reshape(B, S, RET_HEADS, RET_DV)
    o_ret = chunkwise_retention(rq, rk, rv).astype(dt)
    y_ret = jax.nn.silu(r_g) * group_rmsnorm(o_ret, ret_gn_g, RET_HEADS)

    fz = g_f.astype(jnp.float32)
    lbf = lb.astype(jnp.float32)
    f = lbf + (1.0 - lbf) * jax.nn.sigmoid(fz)
    log_f = jnp.log(jnp.maximum(f, MIN_F))
    hk = (1.0 - lbf) * jax.nn.sigmoid(-fz)
    hq = jax.nn.silu(g_q.astype(jnp.float32))
    hi = g_i.astype(jnp.float32)
    o_hg = chunkwise_hgrn2(hq.reshape(B, S, HGRN_HEADS, HGRN_DK), hk.reshape(B, S, HGRN_HEADS, HGRN_DK),
                           hi.reshape(B, S, HGRN_HEADS, HGRN_DV), log_f.reshape(B, S, HGRN_HEADS, HGRN_DK)).astype(dt)
    y_hg = rmsnorm(o_hg, hgrn_norm_g) * jax.nn.silu(g_g)

    merged = jax.nn.sigmoid(m_ret + b_merge[0]) * (y_ret @ w_br_ret) + jax.nn.sigmoid(m_hg + b_merge[1]) * (y_hg @ w_br_hgrn)
    return merged @ w_out


def squared_relu_mlp(h, w_up, w_down):
    a = jax.nn.relu(h @ w_up)
    return (a * a) @ w_down


def setup_inputs(seed: int = 0) -> dict:
    key = jax.random.key(seed)
    ks = jax.random.split(key, 16)
    f32 = jnp.float32

    def w(k, shape, fan_in):
        return jax.random.normal(k, shape, f32) * (fan_in ** -0.5)

    def gain(k, shape):
        return 1.0 + 0.02 * jax.random.normal(k, shape, f32)

    return {
        "x": jax.random.normal(ks[0], (BATCH, SEQ, D_MODEL), f32),
        "norm_mix_g": gain(ks[1], (DEPTH, D_MODEL)),
        "w_in": w(ks[2], (DEPTH, D_MODEL, D_IN), D_MODEL),
        "ret_gn_g": gain(ks[3], (DEPTH, RET_V)),
        "hgrn_norm_g": gain(ks[4], (DEPTH, HGRN_V)),
        "hgrn_lb_logits": 0.1 * jax.random.normal(ks[5], (DEPTH, HGRN_K), f32),
        "w_br_ret": w(ks[6], (DEPTH, RET_V, D_MODEL), RET_V),
        "w_br_hgrn": w(ks[7], (DEPTH, HGRN_V, D_MODEL), HGRN_V),
        "b_merge": 0.02 * jax.random.normal(ks[8], (DEPTH, 2, D_MODEL), f32),
        "w_out": w(ks[9], (DEPTH, D_MODEL, D_MODEL), D_MODEL),
        "norm_ffn_g": gain(ks[10], (DEPTH, D_MODEL)),
        "w_ffn_up": w(ks[11], (DEPTH, D_MODEL, D_FF), D_MODEL),
        "w_ffn_down": w(ks[12], (DEPTH, D_FF, D_MODEL), D_FF),
        "final_norm_g": gain(ks[13], (D_MODEL,)),
    }


def reference(x, norm_mix_g, w_in, ret_gn_g, hgrn_norm_g, hgrn_lb_logits, w_br_ret, w_br_hgrn,
              b_merge, w_out, norm_ffn_g, w_ffn_up, w_ffn_down, final_norm_g):
    lb_sm = jax.nn.softmax(hgrn_lb_logits.astype(jnp.float32), axis=0)
    lower_bounds = jnp.cumsum(lb_sm, axis=0) - lb_sm[0]
    for l in range(DEPTH):
        h = rmsnorm(x, norm_mix_g[l])
        x = x + hybrid_mixer(h, w_in[l], ret_gn_g[l], hgrn_norm_g[l], lower_bounds[l],
                             w_br_ret[l], w_br_hgrn[l], b_merge[l], w_out[l])
        h = rmsnorm(x, norm_ffn_g[l])
        x = x + squared_relu_mlp(h, w_ffn_up[l], w_ffn_down[l])
    return rmsnorm(x, final_norm_g)
```

```python
import contextlib
import numpy as np
import concourse.bass as bass
import concourse.mybir as mybir
from concourse.bass_utils import run_bass_kernel_spmd

F32 = mybir.dt.float32
BF16 = mybir.dt.bfloat16
AF = mybir.ActivationFunctionType
ALU = mybir.AluOpType

D = 1024
DEPTH = 2
NCORES = 8
T = 1024
G = 512
EPS = 1e-6
NSLOT = 3
PH = {"norm", "ret", "hgrn", "merge", "ffn"}
NLAYERS_DBG = DEPTH
DBG = False
CUT = 9
NOROT = False
CSMEM = False
LAST = {}
ENGS = ("pe", "dve", "act", "pool", "sp")


class Buf:
    __slots__ = ("name", "last_w", "readers")

    def __init__(self, name=""):
        self.name = name
        self.last_w = None
        self.readers = []


class Op:
    __slots__ = ("eng", "emit", "deps", "signals", "sem", "val", "stream", "idx")

    def __init__(self, eng, emit, stream, idx):
        self.eng = eng
        self.emit = emit
        self.deps = ()
        self.signals = False
        self.sem = None
        self.val = 0
        self.stream = stream
        self.idx = idx


class Sched:
    def __init__(self, nc):
        self.nc = nc
        self.ops = []

    def add(self, eng, emit, reads=(), writes=(), stream=None):
        op = Op(eng, emit, stream, len(self.ops))
        deps = {}
        for b in reads:
            if b.last_w is not None:
                deps[b.last_w.idx] = b.last_w
        for b in writes:
            if b.last_w is not None:
                deps[b.last_w.idx] = b.last_w
            for r in b.readers:
                deps[r.idx] = r
        if eng == "pe":
            deps = {i: d for i, d in deps.items() if d.eng != "pe"}
        op.deps = tuple(deps.values())
        for d in op.deps:
            d.signals = True
        for b in reads:
            b.readers.append(op)
        for b in writes:
            b.last_w = op
            b.readers = []
        self.ops.append(op)
        return op

    def emit(self, es, final_waits=()):
        nc = self.nc
        esem = {e: es.enter_context(nc.semaphore("s_" + e)) for e in ENGS}
        ssem = {}
        ecount = {e: 0 for e in ENGS}
        scount = {}
        for op in self.ops:
            if op.stream is not None:
                if op.stream not in ssem:
                    ssem[op.stream] = es.enter_context(nc.semaphore("d_" + op.stream))
                    scount[op.stream] = 0
                scount[op.stream] += 16
                op.sem = ssem[op.stream]
                op.val = scount[op.stream]
                op.signals = True
            elif op.signals:
                ecount[op.eng] += 1
                op.sem = esem[op.eng]
                op.val = ecount[op.eng]
        per = {e: [o for o in self.ops if o.eng == e] for e in ENGS}
        block = es.enter_context(nc.Block())

        def run(engname, e):
            waited = {}
            for op in per[engname]:
                need = {}
                for d in op.deps:
                    k = id(d.sem)
                    if waited.get(k, 0) >= d.val:
                        continue
                    if k not in need or need[k][1] < d.val:
                        need[k] = (d.sem, d.val)
                for k, (s, v) in need.items():
                    e.wait_ge(s, v)
                    waited[k] = v
                ins = op.emit(e)
                if op.signals:
                    ins.then_inc(op.sem, 16 if op.stream is not None else 1)
            if engname == "sp":
                for op in final_waits:
                    e.wait_ge(op.sem, op.val)

        @block.tensor
        def _(e):
            run("pe", e)

        @block.vector
        def _(e):
            run("dve", e)

        @block.scalar
        def _(e):
            run("act", e)

        @block.gpsimd
        def _(e):
            run("pool", e)

        @block.sync
        def _(e):
            run("sp", e)


W_IN_COLS = 7168
C_NMIX, C_NFFN, C_NFIN, C_RGN, C_HGN, C_BM, C_LBL, C_KDEC, NCOLS = 0, 16, 32, 40, 56, 64, 96, 104, 112
CM_ID, CM_RMASK, CM_QDEC, CM_BD, CM_M4, CM_RESET, CM_PSW, CM_N = 0, 128, 640, 1152, 1280, 1792, 2304, 2432


def _w_in_perm():
    perm = []
    eo = np.concatenate([np.arange(0, 128, 2), np.arange(1, 128, 2)])
    for h in range(4):
        perm += list(0 + h * 128 + eo)
        perm += list(512 + h * 128 + eo)
        perm += list(1024 + h * 256 + np.arange(256))
        perm += list(2048 + h * 256 + np.arange(256))
    for h in range(4):
        for base in (3072, 3584, 4096, 4608):
            perm += list(base + h * 128 + np.arange(128))
    perm += list(5120 + np.arange(2048))
    return np.asarray(perm, dtype=np.int64)


def _fm(v):
    return np.ascontiguousarray(np.asarray(v, np.float32).reshape(-1, 128).T)


def _const_tables(S):
    f32 = np.float32
    angle = (f32(1.0) / (f32(10000.0) ** np.linspace(0.0, 1.0, 64, dtype=f32))).astype(f32)
    phase = (np.arange(S, dtype=f32)[:, None] * angle[None, :]).astype(f32)
    cos = np.cos(phase).astype(f32).T
    sin = np.sin(phase).astype(f32).T
    cs = np.zeros((128, 2, S), f32)
    cs[:64, 0] = cos
    cs[64:, 0] = cos
    cs[:64, 1] = -sin
    cs[64:, 1] = sin
    cm = np.zeros((128, CM_N), f32)
    cm[:, CM_ID:CM_ID + 128] = np.eye(128, dtype=f32)
    idx = np.arange(128, dtype=np.float64)
    lg = np.log1p(-np.exp2(-5.0 - np.arange(4, dtype=np.float64)))
    kdec = np.zeros((128, 4), f32)
    for h in range(4):
        dist = idx[None, :] - idx[:, None]
        m = np.where(dist >= 0, np.exp(lg[h] * np.maximum(dist, 0.0)), 0.0) * (128.0 ** -0.5)
        cm[:, CM_RMASK + h * 128: CM_RMASK + (h + 1) * 128] = m.astype(f32)
        cm[:, CM_QDEC + h * 128: CM_QDEC + (h + 1) * 128] = np.exp(lg[h] * (idx + 1.0))[None, :].astype(f32)
        kdec[:, h] = (np.exp(lg[h] * (127.0 - idx)) * (128.0 ** -0.5)).astype(f32)
    blk = (idx[:, None] // 32) == (idx[None, :] // 32)
    cm[:, CM_BD:CM_BD + 128] = (blk & (idx[None, :] >= idx[:, None])).astype(f32)
    for c in range(4):
        cm[:, CM_M4 + c * 128: CM_M4 + (c + 1) * 128] = ((idx // 32) == c).astype(f32)[:, None]
    rst = np.ones(512, f32)
    rst[::32] = 0.0
    cm[:, CM_RESET:CM_RESET + 512] = rst[None, :]
    psw = np.zeros((128, 128), f32)
    psw[(np.arange(128) + 64) % 128, np.arange(128)] = 1.0
    cm[:, CM_PSW:CM_PSW + 128] = psw
    chunk_decay = [float(np.exp(lg[h] * 128.0)) for h in range(4)]
    return cs, cm, kdec, chunk_decay


def build_program(nseq, S):
    assert S % T == 0
    nc = bass.Bass("TRN2", target_bir_lowering=False)
    ntok = nseq * S
    x_d = nc.dram_tensor("x", [ntok, D], F32, kind="ExternalInput").ap()
    w_in_d = nc.dram_tensor("w_in", [DEPTH, D, W_IN_COLS], F32, kind="ExternalInput").ap()
    mg_ = "merge" in PH
    ff_ = "ffn" in PH
    w_brr_d = nc.dram_tensor("w_br_ret", [DEPTH, 1024, D] if mg_ else [1, 128, 128], F32, kind="ExternalInput").ap()
    w_brh_d = nc.dram_tensor("w_br_hgrn", [DEPTH, 512, D] if mg_ else [1, 128, 128], F32, kind="ExternalInput").ap()
    w_out_d = nc.dram_tensor("w_out", [DEPTH, D, D] if mg_ else [1, 128, 128], F32, kind="ExternalInput").ap()
    w_up_d = nc.dram_tensor("w_ffn_up", [DEPTH, D, 4096] if ff_ else [1, 128, 128], F32, kind="ExternalInput").ap()
    w_dn_d = nc.dram_tensor("w_ffn_down", [DEPTH, 4096, D] if ff_ else [1, 128, 128], F32, kind="ExternalInput").ap()
    cols_d = nc.dram_tensor("cols", [128, NCOLS], F32, kind="ExternalInput").ap()
    cs_d = nc.dram_tensor("cs", [128, 2, S], F32, kind="ExternalInput").ap()
    cm_d = nc.dram_tensor("cm", [128, CM_N], F32, kind="ExternalInput").ap()
    out_d = nc.dram_tensor("out", [ntok, D], F32, kind="ExternalOutput").ap()
    if DBG:
        dbgR_d = nc.dram_tensor("dbgR", [128, 16 * T], BF16, kind="ExternalOutput").ap()
        dbgZ_d = nc.dram_tensor("dbgZ", [128, 4 * T], BF16, kind="ExternalOutput").ap()
        dbgS_d = nc.dram_tensor("dbgS", [128, T], F32, kind="ExternalOutput").ap()
    _, _, _, chunk_decay = _const_tables(128)

    Sx = Sched(nc)
    es = contextlib.ExitStack()

    def sb(name, shape, dt=F32):
        return es.enter_context(nc.sbuf_tensor(name, shape, dt))

    xT = sb("xT", [128, 8, T]);            xT_b = [Buf("xT0"), Buf("xT1")]
    hT = sb("hT", [128, 8, T], BF16);      hT_b = [Buf("hT0"), Buf("hT1")]
    R = sb("R", [128, 16, T], BF16)
    R_b = [[Buf("R00"), Buf("R01")], [Buf("R10"), Buf("R11")]]
    zT = sb("zT", [128, 4, T], BF16);      zT_b = [Buf("zT0"), Buf("zT1")]
    SSA = sb("SSA", [128, T]);             SSA_b = [Buf("ssa0"), Buf("ssa1")]
    P1 = sb("P1", [128, 2048]);            P1_b = [Buf("P1a"), Buf("P1b")]
    SR_b = [Buf("SRa"), Buf("SRb")]
    T1_b = [Buf("T1a"), Buf("T1b")]
    XIN = [P1[:, 0:1024], P1[:, 1024:2048]]
    SRt = sb("SRt", [128, 4, T], BF16)
    T1t = sb("T1t", [128, 4, T], BF16)
    CS = sb("CS", [128, 2, G]);            CS_b = Buf("CS"); CS2_b = Buf("CS2")
    CM = sb("CM", [128, CM_N]);            CM_b = Buf("CM")
    COLS = sb("COLS", [128, NCOLS]);       COLS_b = Buf("COLS")
    LB = sb("LB", [128, 24]);              LB_b = Buf("LB")
    IDB = sb("IDB", [128, 128], BF16);     IDB_b = Buf("IDB")
    PSWB = sb("PSWB", [128, 128], BF16);   PSWB_b = Buf("PSWB")
    ONES = sb("ONES", [128, 128], BF16);   ONES_b = Buf("ONES")
    RS32 = sb("RS32", [128, DEPTH * 4, 256]);  RS32_b = [[Buf() for _ in range(4)] for _ in range(DEPTH)]
    HS32 = sb("HS32", [128, DEPTH * 4, 128]);  HS32_b = [[Buf() for _ in range(4)] for _ in range(DEPTH)]
    RSB = sb("RSB", [128, 2, 256], BF16);  RSB_b = [Buf("RSB0"), Buf("RSB1")]
    HSB = sb("HSB", [128, 4, 128], BF16);  HSB_b = [Buf("HSB%d" % i) for i in range(4)]
    ring = [sb("ring%d" % i, [128, 4096], BF16) for i in range(NSLOT)]
    ring_b = [Buf("ring%d" % i) for i in range(NSLOT)]
    Fp = [sb("F%d" % i, [128, G]) for i in range(9)];         F_b = [Buf("F%d" % i) for i in range(9)]
    Hp = [sb("H%d" % i, [128, G], BF16) for i in range(6)];   H_b = [Buf("H%d" % i) for i in range(6)]

    class Pool:
        def __init__(self, name, shape, dt, n=2):
            self.t = [sb("%s%d" % (name, i), shape, dt) for i in range(n)]
            self.b = [Buf("%s%d" % (name, i)) for i in range(n)]
            self.i = 0

        def next(self):
            k = self.i % len(self.t)
            self.i += 1
            return self.t[k], self.b[k]

    VBp = Pool("VB", [128, 256], BF16)
    SCMp = Pool("SCM", [128, 128], BF16)
    QDp = Pool("QD", [128, 128], BF16)
    KDp = Pool("KD", [128, 128], BF16)
    OSQp = Pool("OSQ", [128, 256], BF16)
    RSp = Pool("RS", [128, 128], F32)
    YTp = Pool("YT", [128, 256], F32)
    IBp = Pool("IB", [128, 128], BF16)
    KD4p = Pool("KD4", [128, 4, 128], BF16)
    ATTp = Pool("ATT", [128, 128], BF16)

    banks = [es.enter_context(nc.psum_tensor("pb%d" % i, [128, 512], F32)) for i in range(7)]
    bank_b = [Buf("pb%d" % i) for i in range(7)]
    ptb = es.enter_context(nc.psum_tensor("ptb", [128, 8, 128], BF16))
    ptb_b = [Buf("ptb%d" % i) for i in range(8)]
    cnt = {"bank": 0, "pt": 0, "slot": 0, "ob": 0}

    def nb():
        k = cnt["bank"] % 5
        cnt["bank"] += 1
        return banks[k], bank_b[k]

    def nob():
        k = 5 + cnt["ob"] % 2
        cnt["ob"] += 1
        return banks[k], bank_b[k]

    def npt():
        k = cnt["pt"] % 8
        cnt["pt"] += 1
        return ptb[:, k, :], ptb_b[k]

    def MM(out, ob, lhsT, rhs, rb, start=True, stop=True):
        Sx.add("pe", lambda e: e.matmul(out, lhsT=lhsT, rhs=rhs, start=start, stop=stop), reads=rb, writes=[ob])

    def TR(out, ob, in_, ident, rb):
        Sx.add("pe", lambda e: e.transpose(out=out, in_=in_, identity=ident), reads=rb, writes=[ob])

    def ACT(out, ob, in_, rb, func, **kw):
        Sx.add("act", lambda e: e.activation(out=out, in_=in_, func=func, **kw), reads=rb, writes=[ob])

    def TT(out, ob, in0, in1, rb, op, eng="dve"):
        Sx.add(eng, lambda e: e.tensor_tensor(out=out, in0=in0, in1=in1, op=op), reads=rb, writes=[ob])

    def TS(out, ob, in0, s1, s2, op0, op1, rb):
        if s2 is None:
            Sx.add("dve", lambda e: e.tensor_scalar(out=out, in0=in0, scalar1=s1, scalar2=None, op0=op0), reads=rb, writes=[ob])
        else:
            Sx.add("dve", lambda e: e.tensor_scalar(out=out, in0=in0, scalar1=s1, scalar2=s2, op0=op0, op1=op1),
                   reads=rb, writes=[ob])

    def STT(out, ob, in0, scalar, in1, op0, op1, rb):
        Sx.add("dve", lambda e: e.scalar_tensor_tensor(out=out, in0=in0, scalar=scalar, in1=in1, op0=op0, op1=op1),
               reads=rb, writes=[ob])

    def CP(out, ob, in_, rb, eng="dve"):
        Sx.add(eng, lambda e: e.tensor_copy(out=out, in_=in_), reads=rb, writes=[ob])

    def wload(src, kc, ncol):
        k = cnt["slot"] % NSLOT
        cnt["slot"] += 1
        view = ring[k][:, 0:kc * ncol].rearrange("p (k c) -> p k c", k=kc)
        srcv = src.rearrange("(k p) c -> p k c", p=128)
        Sx.add("pool", lambda e: e.dma_start(out=view, in_=srcv), writes=[ring_b[k]], stream="ring%d" % k)
        return view, ring_b[k]

    def col(i):
        return COLS[:, i:i + 1]

    Sx.add("sp", lambda e: e.dma_start(out=CM[:], in_=cm_d), writes=[CM_b], stream="const_cm")
    Sx.add("sp", lambda e: e.dma_start(out=COLS[:], in_=cols_d), writes=[COLS_b], stream="const_cols")
    CP(IDB[:], IDB_b, CM[:, CM_ID:CM_ID + 128], [CM_b])
    CP(PSWB[:], PSWB_b, CM[:, CM_PSW:CM_PSW + 128], [CM_b])
    Sx.add("dve", lambda e: e.memset(ONES[:], 1.0), writes=[ONES_b])
    Sx.add("dve", lambda e: e.memset(LB[:, 0:4], 0.0), writes=[LB_b])
    TT(LB[:, 4:8], LB_b, COLS[:, C_LBL + 4:C_LBL + 8], COLS[:, C_LBL:C_LBL + 4], [COLS_b], ALU.subtract)
    ACT(LB[:, 4:8], LB_b, LB[:, 4:8], [LB_b], AF.Sigmoid)
    TS(LB[:, 8:16], LB_b, LB[:, 0:8], -1.0, 1.0, ALU.mult, ALU.add, [LB_b])
    TS(LB[:, 16:24], LB_b, LB[:, 0:8], 1.0, -1.0, ALU.mult, ALU.add, [LB_b])
    IDF = CM[:, CM_ID:CM_ID + 128]

    def gsl(g):
        return slice(g * G, (g + 1) * G)

    def norm_stats(g, rstd_t, rstd_b):
        bk, bb = nb()
        for c in range(8):
            sq, sqb = Hp[4 + (c % 2)], H_b[4 + (c % 2)]
            ACT(sq[:], sqb, xT[:, c, gsl(g)], [xT_b[g]], AF.Square)
            MM(bk[:], bb, ONES[:], sq[:], [ONES_b, sqb], start=(c == 0), stop=(c == 7))
        ACT(rstd_t[:], rstd_b, bk[:], [bb], AF.Ln, scale=1.0 / D, bias=EPS)
        ACT(rstd_t[:], rstd_b, rstd_t[:], [rstd_b], AF.Exp, scale=-0.5)

    def phase_norm(gain_col0):
        for g in range(2):
            norm_stats(g, Fp[0], F_b[0])
            for c in range(8):
                STT(hT[:, c, gsl(g)], hT_b[g], xT[:, c, gsl(g)], col(gain_col0 + c), Fp[0][:], ALU.mult, ALU.mult,
                    [xT_b[g], COLS_b, F_b[0]])

    def proj_fm(W, Wb, c0, src, src_b, g, kc=8):
        bk, bb = nb()
        for k in range(kc):
            MM(bk[:], bb, W[:, k, c0:c0 + 128], src[:, k, gsl(g)], [Wb, src_b], start=(k == 0), stop=(k == kc - 1))
        return bk, bb

    def ret_post(ob_, obb, tc_, lc, g, l, h):
        OSQ, OSQb = OSQp.next()
        ACT(OSQ[:], OSQb, ob_[:, 0:256], [obb], AF.Square)
        nb_, nbb = nb()
        for vc in range(2):
            MM(nb_[:, 0:128], nbb, ONES[:], OSQ[:, vc * 128:(vc + 1) * 128], [ONES_b, OSQb], start=(vc == 0), stop=(vc == 1))
        RS_, RSb = RSp.next()
        ACT(RS_[:], RSb, nb_[:, 0:128], [nbb], AF.Ln, scale=1.0 / 256, bias=EPS)
        ACT(RS_[:], RSb, RS_[:], [RSb], AF.Exp, scale=-0.5)
        YT, YTb = YTp.next()
        for vc in range(2):
            STT(YT[:, vc * 128:(vc + 1) * 128], YTb, ob_[:, vc * 128:(vc + 1) * 128], col(C_RGN + l * 8 + h * 2 + vc),
                RS_[:], ALU.mult, ALU.mult, [obb, COLS_b, RSb])
        for vc in range(2):
            TT(R[:, h * 2 + vc, tc_], R_b[0][g], YT[:, vc * 128:(vc + 1) * 128], Hp[4 + vc][:, lc],
               [YTb, H_b[4 + vc]], ALU.mult)

    def hg_post(ob_, obb, tc_, lc, g, l, h):
        OSQ, OSQb = OSQp.next()
        ACT(OSQ[:, 0:128], OSQb, ob_[:, 0:128], [obb], AF.Square)
        nb_, nbb = nb()
        MM(nb_[:, 0:128], nbb, ONES[:], OSQ[:, 0:128], [ONES_b, OSQb])
        if h == 0:
            CP(SSA[:, tc_], SSA_b[g], nb_[:, 0:128], [nbb])
        else:
            TT(SSA[:, tc_], SSA_b[g], SSA[:, tc_], nb_[:, 0:128], [SSA_b[g], nbb], ALU.add)
        STT(zT[:, h, tc_], zT_b[g], ob_[:, 0:128], col(C_HGN + l * 4 + h), Hp[0][:, lc], ALU.mult, ALU.mult,
            [obb, COLS_b, H_b[0]])

    out_ops = []
    nblk_seq = S // T
    for blk in range(nseq * nblk_seq):
        seq, bi = divmod(blk, nblk_seq)
        tok0 = seq * S + bi * T
        pos0 = bi * T
        for i in range(8):
            k = i % 2
            xin = XIN[k]
            src = x_d[tok0 + i * 128: tok0 + (i + 1) * 128, :]
            Sx.add("sp", lambda e, xin=xin, src=src: e.dma_start(out=xin, in_=src), writes=[P1_b[k]], stream="p1%d" % k)
            for half in range(2):
                bk, bb = nb()
                for c4 in range(4):
                    c = half * 4 + c4
                    TR(bk[:, c4 * 128:(c4 + 1) * 128], bb, xin[:, c * 128:(c + 1) * 128], IDF, [P1_b[k], CM_b])
                dst = xT[:, half * 4:(half + 1) * 4, i * 128:(i + 1) * 128]
                srcp = bk[:].rearrange("p (c t) -> p c t", c=4)
                if half == 0:
                    CP(dst, xT_b[i // 4], srcp, [bb])
                else:
                    ACT(dst, xT_b[i // 4], srcp, [bb], AF.Copy)
        if bi == 0:
            for l in range(DEPTH):
                for h in range(4):
                    Sx.add("pool", lambda e, l=l, h=h: e.memset(RS32[:, l * 4 + h, :], 0.0), writes=[RS32_b[l][h]])
                    Sx.add("pool", lambda e, l=l, h=h: e.memset(HS32[:, l * 4 + h, :], 0.0), writes=[HS32_b[l][h]])

        for l in range(NLAYERS_DBG):
            if "norm" in PH:
                phase_norm(C_NMIX + l * 8)
            for h in range(4 if "ret" in PH else 0):
                A, Ab = wload(w_in_d[l, :, h * 768: h * 768 + 256], 8, 256)
                B, Bb = wload(w_in_d[l, :, h * 768 + 256: h * 768 + 768], 8, 512)
                S32 = RS32[:, l * 4 + h, :]
                S32b = RS32_b[l][h]
                ACT(RSB[:, 0, :], RSB_b[0], S32, [S32b], AF.Copy)
                pend = [None]
                for g in range(2):
                    p0 = pos0 + g * G
                    if CSMEM:
                        Sx.add("pool", lambda e: e.memset(CS[:, 0, :], 1.0), writes=[CS_b])
                        Sx.add("pool", lambda e: e.memset(CS[:, 1, :], 0.0), writes=[CS2_b])
                    else:
                        Sx.add("sp", lambda e, p0=p0: e.dma_start(out=CS[:, 0, :], in_=cs_d[:, 0, p0:p0 + G]), writes=[CS_b], stream="cs0")
                        Sx.add("sp", lambda e, p0=p0: e.dma_start(out=CS[:, 1, :], in_=cs_d[:, 1, p0:p0 + G]), writes=[CS2_b], stream="cs1")
                    rot = []
                    for w in range(2):
                        bk, bb = proj_fm(A, Ab, w * 128, hT, hT_b[g], g)
                        if NOROT == 1:
                            ACT(Hp[2 + w][:], H_b[2 + w], bk[:], [bb], AF.Copy)
                            continue
                        ACT(Hp[w][:], H_b[w], bk[:], [bb], AF.Copy)
                        ACT(Fp[4][:], F_b[4], bk[:], [bb], AF.Copy)
                        TT(Fp[2][:], F_b[2], CS[:, 0, :], Fp[4][:], [F_b[4], CS_b], ALU.mult)
                        if NOROT == 2:
                            ACT(Hp[2 + w][:], H_b[2 + w], Fp[2][:], [F_b[2]], AF.Copy)
                            continue
                        b2, b2b = nb()
                        MM(b2[:], b2b, PSWB[:], Hp[w][:], [PSWB_b, H_b[w]])
                        if NOROT == 3:
                            ACT(Hp[2 + w][:], H_b[2 + w], b2[:], [b2b], AF.Copy)
                            continue
                        ACT(Fp[5][:], F_b[5], b2[:], [b2b], AF.Copy)
                        TT(Fp[3][:], F_b[3], CS[:, 1, :], Fp[5][:], [F_b[5], CS2_b], ALU.mult)
                        TT(Hp[2 + w][:], H_b[2 + w], Fp[2][:], Fp[3][:], [F_b[2], F_b[3]], ALU.add)
                    QR, QRb, KR, KRb = Hp[2], H_b[2], Hp[3], H_b[3]
                    for vc in range(2):
                        bk, bb = proj_fm(B, Bb, 256 + vc * 128, hT, hT_b[g], g)
                        ACT(Hp[4 + vc][:], H_b[4 + vc], bk[:], [bb], AF.Silu)
                    for i in range(4 if CUT >= 1 else 0):
                        tc_ = slice(g * G + i * 128, g * G + (i + 1) * 128)
                        lc = slice(i * 128, (i + 1) * 128)
                        vb_, vbb = nb()
                        for k in range(8):
                            MM(vb_[:, 0:256], vbb, hT[:, k, tc_], B[:, k, 0:256], [hT_b[g], Bb], start=(k == 0), stop=(k == 7))
                        VB, VBb = VBp.next()
                        ACT(VB[:], VBb, vb_[:, 0:256], [vbb], AF.Copy)
                        if CUT < 2:
                            continue
                        sb_, sbb = nb()
                        MM(sb_[:, 0:128], sbb, KR[:, lc], QR[:, lc], [KRb, QRb])
                        SCM, SCMb = SCMp.next()
                        TT(SCM[:], SCMb, sb_[:, 0:128], CM[:, CM_RMASK + h * 128: CM_RMASK + (h + 1) * 128], [sbb, CM_b], ALU.mult)
                        QD, QDb = QDp.next()
                        TT(QD[:], QDb, QR[:, lc], CM[:, CM_QDEC + h * 128: CM_QDEC + (h + 1) * 128], [QRb, CM_b], ALU.mult)
                        if CUT < 3:
                            continue
                        pt, ptb_ = npt()
                        TR(pt, ptb_, KR[:, lc], IDB[:], [KRb, IDB_b])
                        KD, KDb = KDp.next()
                        STT(KD[:], KDb, pt, col(C_KDEC + h), ONES[:], ALU.mult, ALU.mult, [ptb_, COLS_b, ONES_b])
                        if CUT < 4:
                            continue
                        ob_, obb = nob()
                        cur = (g * 4 + i) % 2
                        for vc in range(2):
                            MM(ob_[:, vc * 128:(vc + 1) * 128], obb, VB[:, vc * 128:(vc + 1) * 128], SCM[:], [VBb, SCMb],
                               start=True, stop=False)
                            MM(ob_[:, vc * 128:(vc + 1) * 128], obb, RSB[:, cur, vc * 128:(vc + 1) * 128], QD[:], [RSB_b[cur], QDb],
                               start=False, stop=True)
                        if pend[0] is not None:
                            pend[0]()
                            pend[0] = None
                        st_, stb = nb()
                        MM(st_[:, 0:256], stb, KD[:], VB[:], [KDb, VBb])
                        STT(RSB[:, 1 - cur, :], RSB_b[1 - cur], S32, chunk_decay[h], st_[:, 0:256], ALU.mult, ALU.add, [S32b, stb])
                        STT(S32, S32b, S32, chunk_decay[h], st_[:, 0:256], ALU.mult, ALU.add, [S32b, stb])
                        pend[0] = (lambda ob_=ob_, obb=obb, tc_=tc_, lc=lc, g=g: ret_post(ob_, obb, tc_, lc, g, l, h))
                    if pend[0] is not None:
                        pend[0]()
                        pend[0] = None
            for h in range(4 if "hgrn" in PH else 0):
                W, Wb = wload(w_in_d[l, :, 3072 + h * 512: 3072 + (h + 1) * 512], 8, 512)
                S32 = HS32[:, l * 4 + h, :]
                S32b = HS32_b[l][h]
                ACT(HSB[:, 0, :], HSB_b[0], S32, [S32b], AF.Copy)
                pend = [None]
                li = l * 4 + h
                for g in range(2):
                    qb, qbb = proj_fm(W, Wb, 0, hT, hT_b[g], g)
                    fb, fbb = proj_fm(W, Wb, 128, hT, hT_b[g], g)
                    gb, gbb = proj_fm(W, Wb, 384, hT, hT_b[g], g)
                    ACT(Fp[1][:], F_b[1], qb[:], [qbb], AF.Silu)
                    ACT(Fp[2][:], F_b[2], fb[:], [fbb], AF.Sigmoid)
                    ACT(Hp[0][:], H_b[0], gb[:], [gbb], AF.Silu)
                    STT(Fp[3][:], F_b[3], Fp[2][:], LB[:, 8 + li:9 + li], LB[:, li:li + 1].to_broadcast([128, G]), ALU.mult, ALU.add, [F_b[2], LB_b])
                    ACT(Fp[3][:], F_b[3], Fp[3][:], [F_b[3]], AF.Ln)
                    Sx.add("dve", lambda e: e.tensor_tensor_scan(out=Fp[4][:], data0=CM[:, CM_RESET:CM_RESET + G], data1=Fp[3][:],
                                                                  initial=0.0, op0=ALU.mult, op1=ALU.add),
                           reads=[CM_b, F_b[3]], writes=[F_b[4]])
                    STT(Fp[5][:], F_b[5], Fp[2][:], LB[:, 16 + li:17 + li], LB[:, 8 + li:9 + li].to_broadcast([128, G]), ALU.mult, ALU.add, [F_b[2], LB_b])
                    ACT(Fp[6][:], F_b[6], Fp[4][:], [F_b[4]], AF.Exp)
                    ACT(Fp[7][:], F_b[7], Fp[4][:], [F_b[4]], AF.Exp, scale=-1.0)
                    TT(Hp[1][:], H_b[1], Fp[1][:], Fp[6][:], [F_b[1], F_b[6]], ALU.mult)
                    TT(Fp[8][:], F_b[8], Fp[5][:], Fp[7][:], [F_b[5], F_b[7]], ALU.mult)
                    ACT(Hp[2][:], H_b[2], Fp[8][:], [F_b[8]], AF.Copy)
                    ebl = Fp[6][:].rearrange("p (c t) -> p c t", t=32)[:, :, 31:32].to_broadcast([128, 16, 32])
                    TT(Hp[3][:].rearrange("p (c t) -> p c t", t=32), H_b[3], Fp[8][:].rearrange("p (c t) -> p c t", t=32), ebl,
                       [F_b[8], F_b[6]], ALU.mult)
                    QE, QEb, KE, KEb, KDT, KDTb, SGG, SGGb, EB, EBb = Hp[1], H_b[1], Hp[2], H_b[2], Hp[3], H_b[3], Hp[0], H_b[0], Fp[6], F_b[6]
                    for i in range(4):
                        tc_ = slice(g * G + i * 128, g * G + (i + 1) * 128)
                        lc = slice(i * 128, (i + 1) * 128)
                        ib_, ibb = nb()
                        for k in range(8):
                            MM(ib_[:, 0:128], ibb, hT[:, k, tc_], W[:, k, 256:384], [hT_b[g], Wb], start=(k == 0), stop=(k == 7))
                        IB, IBb = IBp.next()
                        ACT(IB[:], IBb, ib_[:, 0:128], [ibb], AF.Copy)
                        pt, ptb_ = npt()
                        TR(pt, ptb_, KDT[:, lc], IDB[:], [KDTb, IDB_b])
                        KD4, KD4b = KD4p.next()
                        TT(KD4[:], KD4b, pt.unsqueeze(1).to_broadcast([128, 4, 128]),
                           CM[:, CM_M4:CM_M4 + 512].rearrange("p (c d) -> p c d", c=4), [ptb_, CM_b], ALU.mult)
                        ab_, abb = nb()
                        MM(ab_[:, 0:128], abb, KE[:, lc], QE[:, lc], [KEb, QEb])
                        ATT, ATTb = ATTp.next()
                        TT(ATT[:], ATTb, ab_[:, 0:128], CM[:, CM_BD:CM_BD + 128], [abb, CM_b], ALU.mult)
                        su_, sub = nb()
                        for c in range(4):
                            MM(su_[:, c * 128:(c + 1) * 128], sub, KD4[:, c, :], IB[:], [KD4b, IBb])
                        ob_, obb = nob()
                        MM(ob_[:, 0:128], obb, IB[:], ATT[:], [IBb, ATTb], start=True, stop=False)
                        if pend[0] is not None:
                            pend[0]()
                            pend[0] = None
                        MM(ob_[:, 0:32], obb, HSB[:, 0, :], QE[:, i * 128: i * 128 + 32], [HSB_b[0], QEb], start=False, stop=False)
                        for c in range(4):
                            e_last = EB[:, i * 128 + c * 32 + 31: i * 128 + c * 32 + 32]
                            n_ = (c + 1) % 4
                            STT(HSB[:, n_, :], HSB_b[n_], S32, e_last, su_[:, c * 128:(c + 1) * 128], ALU.mult, ALU.add, [S32b, EBb, sub])
                            STT(S32, S32b, S32, e_last, su_[:, c * 128:(c + 1) * 128], ALU.mult, ALU.add, [S32b, EBb, sub])
                            if c < 3:
                                cc = slice(i * 128 + (c + 1) * 32, i * 128 + (c + 2) * 32)
                                MM(ob_[:, (c + 1) * 32:(c + 2) * 32], obb, HSB[:, c + 1, :], QE[:, cc], [HSB_b[c + 1], QEb],
                                   start=False, stop=(c == 2))
                        pend[0] = (lambda ob_=ob_, obb=obb, tc_=tc_, lc=lc, g=g: hg_post(ob_, obb, tc_, lc, g, l, h))
                    if pend[0] is not None:
                        pend[0]()
                        pend[0] = None
            for g in range(2 if "merge" in PH else 0):
                ACT(SSA[:, gsl(g)], SSA_b[g], SSA[:, gsl(g)], [SSA_b[g]], AF.Ln, scale=1.0 / 512, bias=EPS)
                ACT(SSA[:, gsl(g)], SSA_b[g], SSA[:, gsl(g)], [SSA_b[g]], AF.Exp, scale=-0.5)
            for half in range(2 if "merge" in PH else 0):
                Wm, Wmb = wload(w_in_d[l, :, 5120 + half * 512: 5120 + (half + 1) * 512], 8, 512)
                for f4 in range(4):
                    for g in range(2):
                        bk, bb = proj_fm(Wm, Wmb, f4 * 128, hT, hT_b[g], g)
                        ACT(SRt[:, f4, gsl(g)], SR_b[f4 // 2], bk[:], [bb, COLS_b], AF.Sigmoid,
                            bias=col(C_BM + (l * 2 + 0) * 8 + half * 4 + f4))
                Wr, Wrb = wload(w_brr_d[l, :, half * 512:(half + 1) * 512], 8, 512)
                for f4 in range(4):
                    for g in range(2):
                        bk, bb = proj_fm(Wr, Wrb, f4 * 128, R[:, 0:8, :], R_b[0][g], g)
                        ACT(Fp[4][:], F_b[4], bk[:], [bb], AF.Copy)
                        TT(T1t[:, f4, gsl(g)], T1_b[f4 // 2], SRt[:, f4, gsl(g)], Fp[4][:], [F_b[4], SR_b[f4 // 2]], ALU.mult)
                Wm, Wmb = wload(w_in_d[l, :, 6144 + half * 512: 6144 + (half + 1) * 512], 8, 512)
                for f4 in range(4):
                    for g in range(2):
                        bk, bb = proj_fm(Wm, Wmb, f4 * 128, hT, hT_b[g], g)
                        ACT(SRt[:, f4, gsl(g)], SR_b[f4 // 2], bk[:], [bb, COLS_b], AF.Sigmoid,
                            bias=col(C_BM + (l * 2 + 1) * 8 + half * 4 + f4))
                Wh, Whb = wload(w_brh_d[l, :, half * 512:(half + 1) * 512], 4, 512)
                for f4 in range(4):
                    for g in range(2):
                        bk, bb = proj_fm(Wh, Whb, f4 * 128, zT, zT_b[g], g, kc=4)
                        ACT(Fp[4][:], F_b[4], bk[:], [bb], AF.Copy)
                        TT(Fp[1][:], F_b[1], SSA[:, gsl(g)], Fp[4][:], [F_b[4], SSA_b[g]], ALU.mult)
                        TT(Fp[2][:], F_b[2], Fp[1][:], SRt[:, f4, gsl(g)], [F_b[1], SR_b[f4 // 2]], ALU.mult)
                        TT(R[:, 8 + half * 4 + f4, gsl(g)], R_b[1][g], Fp[2][:], T1t[:, f4, gsl(g)], [F_b[2], T1_b[f4 // 2]], ALU.add)
            for half in range(2 if "merge" in PH else 0):
                Wo, Wob = wload(w_out_d[l, :, half * 512:(half + 1) * 512], 8, 512)
                for f4 in range(4):
                    for g in range(2):
                        bk, bb = proj_fm(Wo, Wob, f4 * 128, R[:, 8:16, :], R_b[1][g], g)
                        c = half * 4 + f4
                        TT(xT[:, c, gsl(g)], xT_b[g], xT[:, c, gsl(g)], bk[:], [xT_b[g], bb], ALU.add)
            if DBG and l == 0 and blk == 0:
                allb = [R_b[0][0], R_b[0][1], R_b[1][0], R_b[1][1], zT_b[0], zT_b[1], SSA_b[0], SSA_b[1]]
                out_ops.append(Sx.add("sp", lambda e: e.dma_start(out=dbgR_d, in_=R[:].rearrange("p a t -> p (a t)")), reads=allb, stream="dbg"))
                out_ops.append(Sx.add("sp", lambda e: e.dma_start(out=dbgZ_d, in_=zT[:].rearrange("p a t -> p (a t)")), reads=allb, stream="dbg"))
                out_ops.append(Sx.add("sp", lambda e: e.dma_start(out=dbgS_d, in_=SSA[:]), reads=allb, stream="dbg"))
            if "ffn" in PH:
                phase_norm(C_NFFN + l * 8)
            for half in range(2 if "ffn" in PH else 0):
                for jb in range(4):
                    Wu, Wub = wload(w_up_d[l, :, half * 2048 + jb * 512: half * 2048 + (jb + 1) * 512], 8, 512)
                    for j4 in range(4):
                        jj = jb * 4 + j4
                        for g in range(2):
                            bk, bb = proj_fm(Wu, Wub, j4 * 128, hT, hT_b[g], g)
                            sq, sqb = Hp[(jj * 2 + g) % 4], H_b[(jj * 2 + g) % 4]
                            ACT(sq[:], sqb, bk[:], [bb], AF.Square)
                            STT(R[:, jj, gsl(g)], R_b[jj // 8][g], bk[:], 0.0, sq[:], ALU.is_gt, ALU.mult, [bb, sqb])
                for fb in range(4):
                    Wd, Wdb = wload(w_dn_d[l, half * 2048:(half + 1) * 2048, fb * 256:(fb + 1) * 256], 16, 256)
                    for f2 in range(2):
                        c = fb * 2 + f2
                        for g in range(2):
                            bk, bb = nb()
                            for jj in range(16):
                                MM(bk[:], bb, Wd[:, jj, f2 * 128:(f2 + 1) * 128], R[:, jj, gsl(g)], [Wdb, R_b[jj // 8][g]],
                                   start=(jj == 0), stop=(jj == 15))
                            TT(xT[:, c, gsl(g)], xT_b[g], xT[:, c, gsl(g)], bk[:], [xT_b[g], bb], ALU.add)
        for g in range(2):
            norm_stats(g, Fp[0], F_b[0])
            for i in range(4):
                ti = g * 4 + i
                k = ti % 2
                tc_ = slice(g * G + i * 128, g * G + (i + 1) * 128)
                lc = slice(i * 128, (i + 1) * 128)
                OTh = [Fp[1 + 2 * k + hh][:].rearrange("p (c t) -> p c t", c=4) for hh in range(2)]
                OTb = [F_b[1 + 2 * k + hh] for hh in range(2)]
                for c in range(8):
                    STT(OTh[c // 4][:, c % 4, :], OTb[c // 4], xT[:, c, tc_], col(C_NFIN + c), Fp[0][:, lc], ALU.mult, ALU.mult,
                        [xT_b[g], COLS_b, F_b[0]])
                xo = XIN[k]
                for half in range(2):
                    bk, bb = nb()
                    for c4 in range(4):
                        TR(bk[:, c4 * 128:(c4 + 1) * 128], bb, OTh[half][:, c4, :], IDF, [OTb[half], CM_b])
                    if half == 0:
                        CP(xo[:, 0:512], P1_b[k], bk[:], [bb])
                    else:
                        ACT(xo[:, 512:1024], P1_b[k], bk[:], [bb], AF.Copy)
                dst = out_d[tok0 + ti * 128: tok0 + (ti + 1) * 128, :]
                out_ops.append(Sx.add("sp", lambda e, xo=xo, dst=dst: e.dma_start(out=dst, in_=xo), reads=[P1_b[k]],
                                      stream="p1%d" % k))
    Sx.emit(es, final_waits=([o for o in out_ops if o.stream == "dbg"][-1:] + out_ops[-2:]) if DBG else out_ops[-2:])
    es.close()
    return nc, len(Sx.ops)


def prepare_shared(inputs, S):
    cs, cm, kdec, _ = _const_tables(S)
    perm = _w_in_perm()
    cols = np.zeros((128, NCOLS), np.float32)
    cols[:, C_NMIX:C_NMIX + 16] = _fm(inputs["norm_mix_g"])
    cols[:, C_NFFN:C_NFFN + 16] = _fm(inputs["norm_ffn_g"])
    cols[:, C_NFIN:C_NFIN + 8] = _fm(inputs["final_norm_g"])
    cols[:, C_RGN:C_RGN + 16] = _fm(inputs["ret_gn_g"])
    cols[:, C_HGN:C_HGN + 8] = _fm(inputs["hgrn_norm_g"])
    cols[:, C_BM:C_BM + 32] = _fm(inputs["b_merge"])
    cols[:, C_LBL:C_LBL + 8] = _fm(inputs["hgrn_lb_logits"])
    cols[:, C_KDEC:C_KDEC + 4] = kdec
    f = lambda a: np.ascontiguousarray(np.asarray(a, np.float32))
    dm = np.zeros((1, 128, 128), np.float32)
    if "merge" not in PH:
        inputs = dict(inputs, w_br_ret=dm, w_br_hgrn=dm, w_out=dm)
    if "ffn" not in PH:
        inputs = dict(inputs, w_ffn_up=dm, w_ffn_down=dm)
    return {
        "w_in": np.ascontiguousarray(np.asarray(inputs["w_in"], np.float32)[:, :, perm]),
        "w_br_ret": f(inputs["w_br_ret"]), "w_br_hgrn": f(inputs["w_br_hgrn"]), "w_out": f(inputs["w_out"]),
        "w_ffn_up": f(inputs["w_ffn_up"]), "w_ffn_down": f(inputs["w_ffn_down"]),
        "cols": cols, "cs": cs, "cm": cm,
    }


_CACHE = {}


def kernel(**inputs):
    x = np.asarray(inputs["x"], np.float32)
    Bt, S, _ = x.shape
    ncores = NCORES if Bt % NCORES == 0 else 1
    nseq = Bt // ncores
    key = (nseq, S)
    if key not in _CACHE:
        _CACHE[key] = build_program(nseq, S)[0]
    nc = _CACHE[key]
    shared = prepare_shared(inputs, S)
    in_maps = []
    for c in range(ncores):
        m = dict(shared)
        m["x"] = np.ascontiguousarray(x[c * nseq:(c + 1) * nseq].reshape(nseq * S, D))
        in_maps.append(m)
    res = run_bass_kernel_spmd(nc, in_maps, core_ids=list(range(ncores)))
    LAST["res"] = res.results
    outs = [np.asarray(r["out"], np.float32).reshape(nseq, S, D) for r in res.results]
    return np.concatenate(outs, axis=0)
```
